# Optimizing a Trainium2 kernel written in Bass

```python
import jax, jax.numpy as jnp
from jax import lax
import numpy as np

D_MODEL = 2048
BATCH = 4
SEQ = 4096
DEPTH = 1

CHUNK = 64

FOX_HEADS = 8
FOX_HEAD_DIM = 128
FOX_WIDTH = FOX_HEADS * FOX_HEAD_DIM
Q_BLOCK = 128
FORGET_BIAS_INIT = 3.0

POOL_WINDOWS = (2, 4, 8, 16)
POOL_GROUPS = len(POOL_WINDOWS)
POOL_WIDTH = 1024
POOL_GROUP_DIM = POOL_WIDTH // POOL_GROUPS

N_BRANCHES = 2
IN_SPLITS = (
    FOX_WIDTH,
    2 * FOX_WIDTH,
    3 * FOX_WIDTH,
    3 * FOX_WIDTH + FOX_HEADS,
    3 * FOX_WIDTH + FOX_HEADS + POOL_WIDTH,
)
IN_COLS = 3 * FOX_WIDTH + FOX_HEADS + POOL_WIDTH + N_BRANCHES * D_MODEL

PEER_HEADS = 8
PEER_KEYS = 128
PEER_EXPERTS = PEER_KEYS * PEER_KEYS
PEER_QUERY_DIM = 256
PEER_HALF = PEER_QUERY_DIM // 2
PEER_TOPK = 16
PEER_TOKEN_BLOCK = 128

RMS_EPS = 1e-6

kernel_name = "hybrid_fox_pool_peer_block"


def rms_norm(x, g):
    xf = x.astype(jnp.float32)
    y = xf * lax.rsqrt(jnp.mean(xf * xf, axis=-1, keepdims=True) + RMS_EPS)
    return (y * g.astype(jnp.float32)).astype(x.dtype)


def forgetting_attention(q, k, v, log_f):
    seq = q.shape[2]
    c = jnp.cumsum(log_f, axis=-1)
    scale = FOX_HEAD_DIM ** -0.5
    outs = []
    for blk in range(seq // Q_BLOCK):
        q0 = blk * Q_BLOCK
        q1 = q0 + Q_BLOCK
        qb = q[:, :, q0:q1]
        kb = k[:, :, :q1]
        vb = v[:, :, :q1]
        logits = jnp.einsum('bhqd,bhkd->bhqk', qb, kb,
                            preferred_element_type=jnp.float32) * scale
        logits = logits + (c[:, :, q0:q1, None] - c[:, :, None, :q1])
        causal = jnp.arange(q0, q1)[:, None] >= jnp.arange(q1)[None, :]
        logits = jnp.where(causal, logits, -jnp.inf)
        p = jax.nn.softmax(logits, axis=-1)
        outs.append(jnp.einsum('bhqk,bhkd->bhqd', p.astype(v.dtype), vb))
    return jnp.concatenate(outs, axis=2)


def multiscale_pool(p, w_groups, scale):
    b, s, _ = p.shape
    pg = p.reshape(b, s, POOL_GROUPS, POOL_GROUP_DIM).astype(jnp.float32)
    csum = jnp.cumsum(pg, axis=1)
    pos = jnp.arange(s)
    outs = []
    for gi, w in enumerate(POOL_WINDOWS):
        cs = csum[:, :, gi]
        lagged = jnp.pad(cs, ((0, 0), (w, 0), (0, 0)))[:, :s]
        count = jnp.minimum(pos + 1, w).astype(jnp.float32)[None, :, None]
        outs.append((cs - lagged) / count - pg[:, :, gi])
    mixed = jnp.stack(outs, axis=2).astype(p.dtype)
    mixed = jnp.einsum('bsgc,gcd->bsgd', mixed, w_groups)
    return mixed.reshape(b, s, POOL_WIDTH) * scale


def peer(xn, w_query, sub_keys_1, sub_keys_2, expert_u, expert_v):
    b, s, d = xn.shape
    t = b * s
    xt = xn.reshape(t, d)
    q = (xt @ w_query).reshape(t, PEER_HEADS, PEER_QUERY_DIM).astype(jnp.float32)
    q1, q2 = q[..., :PEER_HALF], q[..., PEER_HALF:]
    s1 = jnp.einsum('thd,hkd->thk', q1, sub_keys_1.astype(jnp.float32))
    s2 = jnp.einsum('thd,hkd->thk', q2, sub_keys_2.astype(jnp.float32))
    v1, i1 = lax.top_k(s1, PEER_TOPK)
    v2, i2 = lax.top_k(s2, PEER_TOPK)
    cand = (v1[..., :, None] + v2[..., None, :]).reshape(t, PEER_HEADS, PEER_TOPK * PEER_TOPK)
    cand_idx = (i1[..., :, None] * PEER_KEYS + i2[..., None, :]).reshape(
        t, PEER_HEADS, PEER_TOPK * PEER_TOPK)
    top_s, top_pos = lax.top_k(cand, PEER_TOPK)
    expert_idx = jnp.take_along_axis(cand_idx, top_pos, axis=-1)
    gates = jax.nn.softmax(top_s, axis=-1).astype(xn.dtype)

    def token_block(args):
        xb, idx, g = args
        u = jnp.take(expert_u, idx, axis=0)
        a = jnp.einsum('thkd,td->thk', u, xb)
        hid = jax.nn.gelu(a) * g
        vsel = jnp.take(expert_v, idx, axis=0)
        return jnp.einsum('thk,thkd->td', hid, vsel)

    nb = t // PEER_TOKEN_BLOCK
    out = lax.map(token_block, (
        xt.reshape(nb, PEER_TOKEN_BLOCK, d),
        expert_idx.reshape(nb, PEER_TOKEN_BLOCK, PEER_HEADS, PEER_TOPK),
        gates.reshape(nb, PEER_TOKEN_BLOCK, PEER_HEADS, PEER_TOPK),
    ))
    return out.reshape(b, s, d)


def setup_inputs(seed: int = 0) -> dict:
    key = jax.random.key(seed)
    ks = jax.random.split(key, 20)
    f32 = jnp.float32

    def nrm(k, shape, std):
        return jax.random.normal(k, shape, f32) * std

    return {
        "x": nrm(ks[0], (BATCH, SEQ, D_MODEL), 1.0),
        "norm1_g": 1.0 + nrm(ks[1], (DEPTH, D_MODEL), 0.02),
        "w_in": nrm(ks[2], (DEPTH, D_MODEL, IN_COLS), D_MODEL ** -0.5),
        "forget_bias": FORGET_BIAS_INIT + nrm(ks[3], (DEPTH, FOX_HEADS), 0.5),
        "q_norm_g": 1.0 + nrm(ks[4], (DEPTH, FOX_HEAD_DIM), 0.02),
        "k_norm_g": 1.0 + nrm(ks[5], (DEPTH, FOX_HEAD_DIM), 0.02),
        "pool_group_w": nrm(ks[6], (DEPTH, POOL_GROUPS, POOL_GROUP_DIM, POOL_GROUP_DIM),
                            POOL_GROUP_DIM ** -0.5),
        "pool_scale": 1.0 + nrm(ks[7], (DEPTH, POOL_WIDTH), 0.02),
        "w_branch_attn": nrm(ks[8], (DEPTH, FOX_WIDTH, D_MODEL), FOX_WIDTH ** -0.5),
        "w_branch_pool": nrm(ks[9], (DEPTH, POOL_WIDTH, D_MODEL), POOL_WIDTH ** -0.5),
        "w_out": nrm(ks[10], (DEPTH, D_MODEL, D_MODEL), D_MODEL ** -0.5),
        "norm2_g": 1.0 + nrm(ks[11], (DEPTH, D_MODEL), 0.02),
        "peer_w_query": nrm(ks[12], (DEPTH, D_MODEL, PEER_HEADS * PEER_QUERY_DIM), D_MODEL ** -0.5),
        "peer_sub_keys_1": nrm(ks[13], (DEPTH, PEER_HEADS, PEER_KEYS, PEER_HALF), PEER_HALF ** -0.5),
        "peer_sub_keys_2": nrm(ks[14], (DEPTH, PEER_HEADS, PEER_KEYS, PEER_HALF), PEER_HALF ** -0.5),
        "peer_expert_u": nrm(ks[15], (DEPTH, PEER_EXPERTS, D_MODEL), D_MODEL ** -0.5),
        "peer_expert_v": nrm(ks[16], (DEPTH, PEER_EXPERTS, D_MODEL), PEER_HEADS ** -0.5),
    }


def reference(x, norm1_g, w_in, forget_bias, q_norm_g, k_norm_g, pool_group_w, pool_scale,
              w_branch_attn, w_branch_pool, w_out, norm2_g, peer_w_query, peer_sub_keys_1,
              peer_sub_keys_2, peer_expert_u, peer_expert_v):
    b, s, _ = x.shape
    for l in range(DEPTH):
        h = rms_norm(x, norm1_g[l])
        proj = h @ w_in[l]
        q, k, v, fl, pin, gl = jnp.split(proj, IN_SPLITS, axis=-1)

        q = rms_norm(q.reshape(b, s, FOX_HEADS, FOX_HEAD_DIM), q_norm_g[l]).transpose(0, 2, 1, 3)
        k = rms_norm(k.reshape(b, s, FOX_HEADS, FOX_HEAD_DIM), k_norm_g[l]).transpose(0, 2, 1, 3)
        v = v.reshape(b, s, FOX_HEADS, FOX_HEAD_DIM).transpose(0, 2, 1, 3)
        log_f = jax.nn.log_sigmoid(fl.astype(jnp.float32)
                                   + forget_bias[l].astype(jnp.float32)).transpose(0, 2, 1)
        attn = forgetting_attention(q, k, v, log_f)
        y_attn = attn.transpose(0, 2, 1, 3).reshape(b, s, FOX_WIDTH) @ w_branch_attn[l]

        y_pool = multiscale_pool(pin, pool_group_w[l], pool_scale[l]) @ w_branch_pool[l]

        gates = jax.nn.sigmoid(gl.astype(jnp.float32)).astype(x.dtype).reshape(
            b, s, N_BRANCHES, D_MODEL)
        merged = gates[:, :, 0] * y_attn + gates[:, :, 1] * y_pool
        x = x + merged @ w_out[l]

        x = x + peer(rms_norm(x, norm2_g[l]), peer_w_query[l], peer_sub_keys_1[l],
                     peer_sub_keys_2[l], peer_expert_u[l], peer_expert_v[l])
    return x
```

```python
import contextlib
import numpy as np
import concourse.bass as bass
import concourse.mybir as mybir
from concourse.bass_utils import run_bass_kernel_spmd

F32 = mybir.dt.float32
BF16 = mybir.dt.bfloat16
I32 = mybir.dt.int32
U32 = mybir.dt.uint32
AF = mybir.ActivationFunctionType
ALU = mybir.AluOpType
AX = mybir.AxisListType

D = 2048
T = 2048
W = 4096
NT = T // 128
KC = D // 128
NH = 8
IN_COLS = 8200
EPS = 1e-6
NEG = -1.0e30
PEER_TILES = NT
SCRATCH_KIND = "ExternalOutput"
BF16_TABLES = 0
PEER_NB = 4 if BF16_TABLES == 0 else 8


class KB:
    ENGS = ("pe", "act", "dve", "pool", "sp")

    def __init__(self, nc):
        self.nc = nc
        self.es = contextlib.ExitStack()
        self.streams = {e: [] for e in self.ENGS}
        self.sem = {}
        self.cnt = {}
        self.waited = {e: {} for e in self.ENGS}
        self.last_w = {}
        self.readers = {}
        self.uid = 0
        for e in self.ENGS:
            self._mksem("E_" + e)

    def _mksem(self, name):
        if name not in self.sem:
            self.sem[name] = self.es.enter_context(self.nc.semaphore(name))
            self.cnt[name] = 0
        return name

    def sb(self, name, shape, dt, es=None):
        self.uid += 1
        return (es or self.es).enter_context(self.nc.sbuf_tensor(f"{name}_{self.uid}", shape, dt))

    def ps(self, name, shape, dt):
        return self.es.enter_context(self.nc.psum_tensor(name, shape, dt))

    def op(self, eng, fn, reads=(), writes=(), dma_sem=None):
        deps = {}
        for b in reads:
            t = self.last_w.get(b)
            if t is not None and deps.get(t[0], 0) < t[1]:
                deps[t[0]] = t[1]
        for b in writes:
            t = self.last_w.get(b)
            if t is not None and deps.get(t[0], 0) < t[1]:
                deps[t[0]] = t[1]
            for s, v in self.readers.get(b, {}).items():
                if deps.get(s, 0) < v:
                    deps[s] = v
        own = "E_" + eng
        if dma_sem is None:
            self.cnt[own] += 1
            tok = (own, self.cnt[own])
        else:
            self._mksem(dma_sem)
            self.cnt[dma_sem] += 16
            tok = (dma_sem, self.cnt[dma_sem])
        waits = []
        wd = self.waited[eng]
        for s, v in deps.items():
            if s == own and eng == "pe":
                continue
            if wd.get(s, 0) < v:
                waits.append((s, v))
                wd[s] = v
        self.streams[eng].append((waits, fn, tok))
        for b in reads:
            r = self.readers.setdefault(b, {})
            if r.get(tok[0], 0) < tok[1]:
                r[tok[0]] = tok[1]
        for b in writes:
            self.last_w[b] = tok
            self.readers[b] = {}
        return tok

    def barrier(self):
        snap = dict(self.cnt)
        for e in self.ENGS:
            waits = []
            for s, v in snap.items():
                if v > 0 and self.waited[e].get(s, 0) < v and not (s == "E_" + e):
                    waits.append((s, v))
                    self.waited[e][s] = v
            if waits:
                self.streams[e].append((waits, None, None))

    def finish(self):
        nc = self.nc
        self.barrier()
        streams = self.streams
        sem = self.sem

        def replay(engname, eng):
            for waits, fn, tok in streams[engname]:
                for s, v in waits:
                    eng.wait_ge(sem[s], v)
                if fn is None:
                    continue
                ins = fn(eng)
                ins.then_inc(sem[tok[0]], 1 if tok[0][:2] == "E_" else 16)

        with nc.Block() as block:
            @block.tensor
            def _(e):
                replay("pe", e)

            @block.scalar
            def _(e):
                replay("act", e)

            @block.vector
            def _(e):
                replay("dve", e)

            @block.gpsimd
            def _(e):
                replay("pool", e)

            @block.sync
            def _(e):
                replay("sp", e)
        self.es.close()

    def dma(self, q, out, in_, reads, writes, sem, **kw):
        return self.op(q, lambda e: e.dma_start(out=out, in_=in_, **kw), reads, writes, dma_sem=sem)

    def mm(self, out, lhsT, rhs, start, stop, reads, writes):
        return self.op("pe", lambda e: e.matmul(out, lhsT=lhsT, rhs=rhs, start=start, stop=stop), reads, writes)

    def tr(self, out, in_, ident, reads, writes):
        return self.op("pe", lambda e: e.transpose(out=out, in_=in_, identity=ident), reads, writes)

    def act(self, out, in_, func, reads, writes, eng="act", **kw):
        return self.op(eng, lambda e: e.activation(out=out, in_=in_, func=func, **kw), reads, writes)

    def copy(self, eng, out, in_, reads, writes):
        if eng == "act":
            return self.op(eng, lambda e: e.copy(out=out, in_=in_), reads, writes)
        return self.op(eng, lambda e: e.tensor_copy(out=out, in_=in_), reads, writes)

    def tt(self, eng, out, in0, in1, op, reads, writes):
        return self.op(eng, lambda e: e.tensor_tensor(out=out, in0=in0, in1=in1, op=op), reads, writes)

    def ts(self, eng, out, in0, s1, s2, op0, op1, reads, writes):
        if op1 is None:
            return self.op(eng, lambda e: e.tensor_single_scalar(out=out, in_=in0, scalar=s1, op=op0), reads, writes)
        return self.op(eng, lambda e: e.tensor_scalar(out=out, in0=in0, scalar1=s1, scalar2=s2, op0=op0, op1=op1),
                       reads, writes)

    def stt(self, eng, out, in0, scalar, in1, op0, op1, reads, writes, **kw):
        return self.op(eng, lambda e: e.scalar_tensor_tensor(out=out, in0=in0, scalar=scalar, in1=in1,
                                                             op0=op0, op1=op1, **kw), reads, writes)

    def memset(self, eng, ap, val, writes):
        return self.op(eng, lambda e: e.memset(ap, val), (), writes)


def build_program(debug=False, upto=99):
    nc = bass.Bass("TRN2", target_bir_lowering=False)
    IK = "ExternalOutput" if debug else SCRATCH_KIND

    def din(name, shape, dt=F32):
        return nc.dram_tensor(name, shape, dt, kind="ExternalInput").ap()

    x_own = din("x_own", [T, D])
    x_prev = din("x_prev", [T, D])
    meta = din("meta", [128, 2])
    norm1_g = din("norm1_g", [1, D])
    w_in = din("w_in", [D, IN_COLS])
    forget_bias = din("forget_bias", [NH, 1])
    q_norm_g = din("q_norm_g", [128, 1])
    k_norm_g = din("k_norm_g", [128, 1])
    pool_group_w = din("pool_group_w", [4, 256, 256])
    pool_scale = din("pool_scale", [8, 128])
    w_battn = din("w_branch_attn", [1024, D])
    w_bpool = din("w_branch_pool", [1024, D])
    w_out = din("w_out", [D, D])
    norm2_g = din("norm2_g", [1, D])
    w_query = din("peer_w_query", [D, D])
    sk1 = din("peer_sub_keys_1", [NH, 128, 128])
    sk2 = din("peer_sub_keys_2", [NH, 128, 128])
    exp_u = din("peer_expert_u", [16385, D])
    exp_v = din("peer_expert_v", [16385, D])
    out_d = nc.dram_tensor("out", [T, D], F32, kind="ExternalOutput").ap()
    if BF16_TABLES == 1:
        exp_u_bf = exp_u.bitcast(BF16).rearrange("r (two c) -> (r two) c", two=2)
        exp_v_bf = exp_v.bitcast(BF16).rearrange("r (two c) -> (r two) c", two=2)
    elif BF16_TABLES == 2:
        exp_u_bf = nc.dram_tensor("s_Ubf", [16384, D], BF16, kind="ExternalOutput").ap()
        exp_v_bf = nc.dram_tensor("s_Vbf", [16384, D], BF16, kind="ExternalOutput").ap()
    else:
        exp_u_bf, exp_v_bf = exp_u, exp_v
    GDT = BF16 if BF16_TABLES else F32

    QT = nc.dram_tensor("s_QT", [NH, 128, T], BF16, kind=IK).ap()
    KT = nc.dram_tensor("s_KT", [NH, 128, W], BF16, kind=IK).ap()
    VS = nc.dram_tensor("s_VS", [NH, W, 128], BF16, kind=IK).ap()
    GT = nc.dram_tensor("s_GT", [2, KC, 128, T], BF16, kind=IK).ap()
    PTd = nc.dram_tensor("s_PT", [128, 8, T], BF16, kind=IK).ap()
    X1 = out_d
    if debug:
        dbg_negc = nc.dram_tensor("d_negc", [NH, W], F32, kind="ExternalOutput").ap()
        dbg_attnT = nc.dram_tensor("d_attnT", [128, NH, T], BF16, kind="ExternalOutput").ap()
        dbg_mixT = nc.dram_tensor("d_mixT", [128, 8, T], BF16, kind="ExternalOutput").ap()
        dbg_idx = nc.dram_tensor("d_idx", [128, NT, 128], I32, kind="ExternalOutput").ap()
        dbg_gate = nc.dram_tensor("d_gate", [128, NT, 128], F32, kind="ExternalOutput").ap()
        dbg_mrg = nc.dram_tensor("d_mrg", [128, KC, T], BF16, kind="ExternalOutput").ap()

    k = KB(nc)
    w_in_v = w_in.rearrange("(kc p) c -> p kc c", p=128)

    M = [k.ps(f"M{i}", [128, 512], F32) for i in range(4)]
    TP = [k.ps(f"TP{i}", [128, 2048], BF16) for i in range(2)]

    identf = k.sb("identf", [128, 128], F32)
    ident = k.sb("ident", [128, 128], BF16)
    onesf = k.sb("onesf", [128, 128], F32)
    ones_bf = k.sb("ones_bf", [128, 128], BF16)
    mask01 = k.sb("mask01", [128, 128], BF16)
    meta_sb = k.sb("meta_sb", [128, 2], F32)
    gq = k.sb("gq", [128, 1], F32)
    gk = k.sb("gk", [128, 1], F32)
    negfb = k.sb("negfb", [NH, 1], F32)
    pscale = k.sb("pscale", [128, 8], F32)

    k.memset("pool", identf[:], 0.0, ["identf"])
    k.op("pool", lambda e: e.affine_select(out=identf[:], in_=identf[:], pattern=[[-1, 128]],
                                           compare_op=ALU.not_equal, fill=1.0, base=0, channel_multiplier=1),
         ["identf"], ["identf"])
    k.copy("dve", ident[:], identf[:], ["identf"], ["ident"])
    k.memset("pool", onesf[:], 1.0, ["onesf"])
    k.copy("dve", ones_bf[:], onesf[:], ["onesf"], ["ones_bf"])
    k.op("pool", lambda e: e.affine_select(out=onesf[:], in_=onesf[:], pattern=[[1, 128]],
                                           compare_op=ALU.is_ge, fill=0.0, base=0, channel_multiplier=-1),
         ["onesf"], ["onesf"])
    k.copy("dve", mask01[:], onesf[:], ["onesf"], ["mask01"])
    k.dma("sp", meta_sb[:], meta, [], ["meta"], "d_c0")
    k.dma("sp", gq[:], q_norm_g, [], ["gq"], "d_c1")
    k.dma("sp", gk[:], k_norm_g, [], ["gk"], "d_c2")
    k.dma("sp", negfb[:], forget_bias, [], ["negfb"], "d_c3")
    k.dma("sp", pscale[:], pool_scale.rearrange("c p -> p c"), [], ["pscale"], "d_c4",
          allow_slow_non_contiguous=True)
    k.ts("dve", gq[:], gq[:], float(128 ** -0.5), None, ALU.mult, None, ["gq"], ["gq"])
    k.ts("dve", negfb[:], negfb[:], -1.0, None, ALU.mult, None, ["negfb"], ["negfb"])

    S_L = contextlib.ExitStack()
    lT = k.sb("lT", [NH, W], F32, S_L)
    S_X = contextlib.ExitStack()
    pin_prev = k.sb("pin_prev", [128, 8, 128], F32, S_X)
    mixT = k.sb("mixT", [128, 8, T], BF16, S_X)

    with contextlib.ExitStack() as es:
        hT = k.sb("hT", [128, KC, T], BF16, es)
        wl_state = {"n": 0}

        def issue_w(p):
            c0, ncols = wl_state["plan"][p]
            i = p % 2
            k.dma("pool", Wbuf[i][:, :, 0:ncols], w_in_v[:, :, c0:c0 + ncols], [], [f"Wbuf{i}"], f"d_W{i}")

        def load_w(c0, ncols):
            p = wl_state["n"]
            assert wl_state["plan"][p] == (c0, ncols)
            if wl_state["issued"] <= p:
                issue_w(p)
                wl_state["issued"] = p + 1
            if p + 1 < len(wl_state["plan"]) and wl_state["issued"] <= p + 1:
                issue_w(p + 1)
                wl_state["issued"] = p + 2
            wl_state["n"] += 1
            return p % 2

        mrot = {"n": 0}

        def next_banks():
            i = mrot["n"] % 2
            mrot["n"] += 1
            return 2 * i, 2 * i + 1

        obn = {"n": 0}

        for half in range(2):
            xsrc = x_prev if half == 0 else x_own
            wt0 = half * T
            with contextlib.ExitStack() as es2:
                xin = [k.sb(f"xin{i}", [128, D], F32, es2) for i in range(2)]
                g_bc = k.sb("g_bc", [128, D], F32, es2)
                hbf = [k.sb(f"hbf{i}", [128, D], BF16, es2) for i in range(2)]
                junkb = k.sb("junkb", [128, D], BF16, es2)
                ss = [k.sb(f"ss{i}", [128, 1], F32, es2) for i in range(2)]
                k.dma("sp", g_bc[:], norm1_g.partition_broadcast(128), [], ["g_bc"], "d_gbc")
                k.dma("sp", xin[0][:], xsrc[0:128, :], [], ["xin0"], "d_xin0")
                for tt in range(NT):
                    b = tt % 2
                    if tt + 1 < NT:
                        nb = (tt + 1) % 2
                        k.dma("sp", xin[nb][:], xsrc[(tt + 1) * 128:(tt + 2) * 128, :], [], [f"xin{nb}"], f"d_xin{nb}")
                    k.memset("dve", ss[b][:], 0.0, [f"ss{b}"])
                    k.act(junkb[:], xin[b][:], AF.Square, [f"xin{b}", f"ss{b}"], ["junkb", f"ss{b}"], accum_out=ss[b][:])
                    k.act(ss[b][:], ss[b][:], AF.Sqrt, [f"ss{b}"], [f"ss{b}"], bias=EPS, scale=1.0 / D)
                    k.op("dve", lambda e, b=b, ss=ss: e.reciprocal(out=ss[b][:], in_=ss[b][:]), [f"ss{b}"], [f"ss{b}"])
                    k.stt("dve", hbf[b][:], xin[b][:], ss[b][:, 0:1], g_bc[:], ALU.mult, ALU.mult,
                          [f"xin{b}", f"ss{b}", "g_bc"], [f"hbf{b}"])
                    for c in range(KC):
                        k.tr(TP[b][:, c * 128:(c + 1) * 128], hbf[b][:, c * 128:(c + 1) * 128], ident[:],
                             [f"hbf{b}", "ident"], [f"TP{b}"])
                    k.copy("act", hT[:, :, tt * 128:(tt + 1) * 128],
                           TP[b][:].rearrange("p (c t) -> p c t", c=KC), [f"TP{b}"], [("hT", tt)])
            k.barrier()
            esB = contextlib.ExitStack()
            Wbuf = [k.sb(f"Wbuf{i}", [128, KC, 512], BF16, esB) for i in range(2)]
            plan = [(1024, 512), (1536, 512), (2048, 512), (2560, 512), (3072, 8), (3080, 512), (3592, 512)]
            if half == 1:
                plan += [(0, 512), (512, 512)] + [(4104 + 512 * i_, 512) for i_ in range(8)]
            wl_state.update(n=0, issued=0, plan=plan)
            sqb = [k.sb(f"sqb{i}", [128, 512], BF16, esB) for i in range(2)]
            rsb = [k.sb(f"rsb{i}", [128, 512], F32, esB) for i in range(2)]
            ob = [k.sb(f"ob{i}", [128, 512], BF16, esB) for i in range(3)]
            e8 = [k.sb(f"e8{i}", [NH, 512], F32, esB) for i in range(2)]
            pchunk = [k.sb(f"pchunk{i}", [128, 128 + T], F32, esB) for i in range(1)]
            wtmp = [k.sb(f"wtmp{i}", [128, 128 + T], F32, esB) for i in range(2)]
            invc = k.sb("invc", [128, T], F32, esB)

            hT_g = lambda tg: [("hT", 4 * tg + i) for i in range(4)]

            def qk_unit(wi, sub, tg, gcol, gkey, dst, which):
                m0, m1 = next_banks()
                for c in range(KC):
                    k.mm(M[m0][:], Wbuf[wi][:, c, sub * 128:(sub + 1) * 128], hT[:, c, tg * 512:(tg + 1) * 512],
                         c == 0, c == KC - 1, [f"Wbuf{wi}"] + hT_g(tg), [f"M{m0}"])
                sb_ = sqb[mrot["n"] % 2]
                sk = f"sqb{mrot['n'] % 2}"
                rb = rsb[mrot["n"] % 2]
                rk = f"rsb{mrot['n'] % 2}"
                k.act(sb_[:], M[m0][:], AF.Square, [f"M{m0}"], [sk])
                k.mm(M[m1][:], ones_bf[:], sb_[:], True, True, [sk, "ones_bf"], [f"M{m1}"])
                k.act(rb[:], M[m1][:], AF.Sqrt, [f"M{m1}"], [rk], bias=EPS, scale=1.0 / 128)
                k.op("dve", lambda e, rb=rb: e.reciprocal(out=rb[:], in_=rb[:]), [rk], [rk])
                oi = obn["n"] % 3
                obn["n"] += 1
                k.stt("dve", ob[oi][:], M[m0][:], gcol[:, 0:1], rb[:], ALU.mult, ALU.mult,
                      [f"M{m0}", rk, gkey], [f"ob{oi}"])
                k.dma("sp", dst, ob[oi][:], [f"ob{oi}"], [], f"d_ob{oi}")

            for wl in range(2):
                wi = load_w(1024 + wl * 512, 512)
                for sub in range(4):
                    h = wl * 4 + sub
                    for tg in range(4):
                        qk_unit(wi, sub, tg, gk, "gk", KT[h, :, wt0 + tg * 512: wt0 + (tg + 1) * 512], "KT")
            for wl in range(2):
                wi = load_w(2048 + wl * 512, 512)
                for tt in range(NT):
                    m0, _ = next_banks()
                    for c in range(KC):
                        k.mm(M[m0][:], hT[:, c, tt * 128:(tt + 1) * 128], Wbuf[wi][:, c, :],
                             c == 0, c == KC - 1, [f"Wbuf{wi}", ("hT", tt)], [f"M{m0}"])
                    oi = obn["n"] % 3
                    obn["n"] += 1
                    k.copy("act", ob[oi][:], M[m0][:], [f"M{m0}"], [f"ob{oi}"])
                    k.dma("sp", VS[wl * 4:(wl + 1) * 4, wt0 + tt * 128: wt0 + (tt + 1) * 128, :].rearrange("h t d -> t h d"),
                          ob[oi][:].rearrange("p (h d) -> p h d", h=4), [f"ob{oi}"], [], f"d_ob{oi}")
            wi = load_w(3072, 8)
            for tg in range(4):
                m0, _ = next_banks()
                for c in range(KC):
                    k.mm(M[m0][0:NH, :], Wbuf[wi][:, c, 0:NH], hT[:, c, tg * 512:(tg + 1) * 512],
                         c == 0, c == KC - 1, [f"Wbuf{wi}"] + hT_g(tg), [f"M{m0}"])
                eb = tg % 2
                k.act(e8[eb][:], M[m0][0:NH, :], AF.Exp, [f"M{m0}", "negfb"], [f"e8{eb}"], bias=negfb[:, 0:1], scale=-1.0)
                k.act(lT[:, wt0 + tg * 512: wt0 + (tg + 1) * 512], e8[eb][:], AF.Ln, [f"e8{eb}"], ["lT"], bias=1.0, scale=1.0)
            for wl in range(2):
                wi = load_w(3080 + wl * 512, 512)
                for sub in range(4):
                    c8 = wl * 4 + sub
                    if half == 0:
                        m0, _ = next_banks()
                        for c in range(KC):
                            k.mm(M[m0][:, 0:128], Wbuf[wi][:, c, sub * 128:(sub + 1) * 128], hT[:, c, T - 128:T],
                                 c == 0, c == KC - 1, [f"Wbuf{wi}", ("hT", NT - 1)], [f"M{m0}"])
                        k.copy("act", pin_prev[:, c8, :], M[m0][:, 0:128], [f"M{m0}"], ["pin_prev"])
                        continue
                    pb = 0
                    pk = f"pchunk{pb}"
                    k.copy("pool", pchunk[pb][:, 0:128], pin_prev[:, c8, :], ["pin_prev"], [pk])
                    for tg in range(4):
                        m0, _ = next_banks()
                        for c in range(KC):
                            k.mm(M[m0][:], Wbuf[wi][:, c, sub * 128:(sub + 1) * 128], hT[:, c, tg * 512:(tg + 1) * 512],
                                 c == 0, c == KC - 1, [f"Wbuf{wi}"] + hT_g(tg), [f"M{m0}"])
                        k.copy("act", pchunk[pb][:, 128 + tg * 512: 128 + (tg + 1) * 512], M[m0][:], [f"M{m0}"], [pk])
                    g = c8 // 2
                    wlen = (2, 4, 8, 16)[g]
                    if c8 % 2 == 0:
                        k.op("pool", lambda e, invc=invc: e.iota(invc[:], pattern=[[1, T]], base=1, channel_multiplier=0,
                                                      allow_small_or_imprecise_dtypes=True), [], ["invc"])
                        k.ts("dve", invc[:], invc[:], meta_sb[:, 1:2], float(wlen), ALU.add, ALU.min, ["invc", "meta"], ["invc"])
                        k.op("dve", lambda e, invc=invc: e.reciprocal(out=invc[:], in_=invc[:]), ["invc"], ["invc"])
                    src, sk_ = pchunk[pb], pk
                    lo, sh, lvl = 1, 1, 0
                    NTOT = 128 + T
                    while sh < wlen:
                        dst_, dk_ = wtmp[lvl % 2], f"wtmp{lvl % 2}"
                        k.tt("pool" if lvl % 2 else "dve", dst_[:, lo:NTOT], src[:, lo:NTOT], src[:, lo - sh:NTOT - sh],
                             ALU.add, [sk_], [dk_])
                        src, sk_ = dst_, dk_
                        sh *= 2
                        lo = 2 * lo + 1 if lvl else 3
                        lvl += 1
                    fin, fk = wtmp[lvl % 2], f"wtmp{lvl % 2}"
                    k.tt("dve", fin[:, 128:NTOT], src[:, 128:NTOT], invc[:], ALU.mult, [sk_, "invc"], [fk])
                    k.tt("pool", mixT[:, c8, :], fin[:, 128:NTOT], pchunk[pb][:, 128:NTOT], ALU.subtract, [fk, pk], [("mixT", c8)])
            if half == 0:
                k.barrier()
                esB.close()
                continue
            for wl in range(2):
                wi = load_w(wl * 512, 512)
                for sub in range(4):
                    h = wl * 4 + sub
                    for tg in range(4):
                        qk_unit(wi, sub, tg, gq, "gq", QT[h, :, tg * 512:(tg + 1) * 512], "QT")
            for wl in range(8):
                wi = load_w(4104 + wl * 512, 512)
                for sub in range(4):
                    cc = wl * 4 + sub
                    br, ch = cc // KC, cc % KC
                    for tg in range(4):
                        m0, _ = next_banks()
                        for c in range(KC):
                            k.mm(M[m0][:], Wbuf[wi][:, c, sub * 128:(sub + 1) * 128], hT[:, c, tg * 512:(tg + 1) * 512],
                                 c == 0, c == KC - 1, [f"Wbuf{wi}"] + hT_g(tg), [f"M{m0}"])
                        oi = obn["n"] % 3
                        obn["n"] += 1
                        k.act(ob[oi][:], M[m0][:], AF.Sigmoid, [f"M{m0}"], [f"ob{oi}"])
                        k.dma("sp", GT[br, ch, :, tg * 512:(tg + 1) * 512], ob[oi][:], [f"ob{oi}"], [], f"d_ob{oi}")
            k.barrier()
            esB.close()
    if debug:
        k.dma("sp", dbg_mixT, mixT[:], [("mixT", c) for c in range(8)], [], "d_dbg")
    with contextlib.ExitStack() as es:
        Wg = k.sb("Wg", [128, 8, 256], BF16, es)
        pob = [k.sb(f"pob{i}", [128, 512], BF16, es) for i in range(3)]
        k.dma("pool", Wg[:], pool_group_w.rearrange("g (kc p) d -> p (g kc) d", p=128), [], ["Wg"], "d_Wg")
        n = 0
        for g in range(4):
            for dch in range(2):
                for tg in range(4):
                    m0 = n % 4
                    o3 = n % 3
                    n += 1
                    for kc2 in range(2):
                        k.mm(M[m0][:], Wg[:, g * 2 + kc2, dch * 128:(dch + 1) * 128], mixT[:, g * 2 + kc2, tg * 512:(tg + 1) * 512],
                             kc2 == 0, kc2 == 1, ["Wg", ("mixT", g * 2 + kc2)], [f"M{m0}"])
                    c8 = g * 2 + dch
                    k.act(pob[o3][:], M[m0][:], AF.Copy, [f"M{m0}", "pscale"], [f"pob{o3}"], scale=pscale[:, c8:c8 + 1])
                    k.dma("sp", PTd[:, c8, tg * 512:(tg + 1) * 512], pob[o3][:], [f"pob{o3}"], [], f"d_pob{o3}")
        k.barrier()
    S_X.close()
    if upto <= 1:
        k.finish()
        return nc

    S_G = contextlib.ExitStack()
    mergedT = k.sb("mergedT", [128, KC, T], BF16, S_G)
    S_A = contextlib.ExitStack()
    attnT = k.sb("attnT", [128, NH, T], BF16, S_A)
    S_B = contextlib.ExitStack()
    Btab = k.sb("Btab", [128, NH, 32, NT], F32, S_B)
    with contextlib.ExitStack() as es:
        negc = k.sb("negc", [NH, W], F32, es)
        zer = k.sb("zer", [NH, W], F32, es)
        negc_tok = k.sb("negc_tok", [128, 32, NH], F32, es)
        qend_bc = k.sb("qend_bc", [128, NH, NT], F32, es)
        sel = k.sb("sel", [NH, NH, 128], F32, es)
        k.memset("dve", zer[:], 0.0, ["zer"])
        k.op("dve", lambda e: e.tensor_tensor_scan(out=negc[:], data0=lT[:], data1=zer[:], initial=0.0,
                                                   op0=ALU.add, op1=ALU.add), ["lT", "zer"], ["negc"])
        if debug:
            k.dma("sp", dbg_negc, negc[:], ["negc"], [], "d_dbg")
        for kt in range(32):
            k.tr(M[0][:, kt * NH:(kt + 1) * NH], negc[0:NH, kt * 128:(kt + 1) * 128], identf[0:NH, 0:NH],
                 ["negc", "identf"], ["M0"])
        k.copy("dve", negc_tok[:], M[0][:, 0:32 * NH].rearrange("p (k h) -> p k h", h=NH), ["M0"], ["negc_tok"])
        k.copy("dve", sel[:], identf[0:NH, 0:NH].unsqueeze(2).to_broadcast([NH, NH, 128]), ["identf"], ["sel"])
        for h in range(NH):
            k.mm(M[1][:, h * NT:(h + 1) * NT], sel[:, h, :], negc[0:NH, T + 127:W:128], True, True,
                 ["sel", "negc"], ["M1"])
        k.copy("dve", qend_bc[:], M[1][:, 0:NH * NT].rearrange("p (h q) -> p h q", q=NT), ["M1"], ["qend_bc"])
        for h in range(NH):
            k.tt("dve", Btab[:, h], negc_tok[:, :, h].unsqueeze(2).to_broadcast([128, 32, NT]),
                 qend_bc[:, h, :].unsqueeze(1).to_broadcast([128, 32, NT]), ALU.subtract,
                 ["negc_tok", "qend_bc"], ["Btab"])
        k.barrier()
    if upto <= 2:
        k.finish()
        return nc

    with contextlib.ExitStack() as es:
        KTh = [k.sb(f"KTh{i}", [128, W], BF16, es) for i in range(2)]
        Vh = [k.sb(f"Vh{i}", [128, 32, 130], BF16, es) for i in range(2)]
        QTh = [k.sb(f"QTh{i}", [128, T], BF16, es) for i in range(2)]
        NPT = 8
        ptb = [k.sb(f"ptb{i}", [128, 128], BF16, es) for i in range(NPT)]
        rden = [k.sb(f"rden{i}", [128, 1], F32, es) for i in range(2)]
        atile = [k.sb(f"atile{i}", [128, 128], BF16, es) for i in range(2)]
        for i in range(2):
            k.memset("dve", Vh[i][:, :, 128:130], 1.0, [f"Vh{i}"])
            k.copy("dve", Vh[i][:, 0:16, 128:129], meta_sb[:, 0:1].unsqueeze(1).to_broadcast([128, 16, 1]),
                   ["meta", f"Vh{i}"], [f"Vh{i}"])

        def load_head(h):
            i = h % 2
            k.dma("sp", KTh[i][:], KT[h], ["KT"], [f"KTh{i}"], f"d_KTh{i}")
            k.dma("sp", Vh[i][:, :, 0:128], VS[h].rearrange("(kt p) d -> p kt d", p=128), ["VS"], [f"Vh{i}"], f"d_Vh{i}")
            k.dma("sp", QTh[i][:], QT[h], ["QT"], [f"QTh{i}"], f"d_QTh{i}")

        cb = [k.sb(f"cb{i}", [128, D], BF16, es) for i in range(2)]
        cstate = {"n": 0}

        def conv_steps(cnt):
            for _ in range(cnt):
                n_ = cstate["n"]
                if n_ >= 256:
                    return
                cstate["n"] += 1
                src, dstv = (exp_u, exp_u_bf) if n_ < 128 else (exp_v, exp_v_bf)
                r = n_ % 128
                b_ = n_ % 2
                k.dma("pool", cb[b_][:], src[r * 128:(r + 1) * 128, :], [], [f"cb{b_}"], f"d_cb{b_}")
                k.dma("sp", dstv[r * 128:(r + 1) * 128, :], cb[b_][:], [f"cb{b_}", f"cb{1 - b_}"], [], f"d_cs{b_}")

        TP1f = TP[1][:].bitcast(F32)

        def oacc(j_):
            if j_ == 0:
                return M[2][:, 0:129], ("M2", 0)
            if j_ == 1:
                return M[3][:, 0:129], ("M3", 0)
            if j_ == 2:
                return TP1f[:, 0:129], ("TP1", 0)
            return TP1f[:, 512:641], ("TP1", 1)

        load_head(0)
        pn = 0
        sn = 0
        an = 0
        for h in range(NH):
            i = h % 2
            if h + 1 < NH:
                load_head(h + 1)
            for qg in range(4):
                if BF16_TABLES:
                    conv_steps(8)
                nkt = 16 + 4 * qg + 4
                def emit_S(kt_, sbk_):
                    k.mm(M[sbk_][:], KTh[i][:, kt_ * 128:(kt_ + 1) * 128], QTh[i][:, qg * 512:(qg + 1) * 512], True, True,
                         [f"KTh{i}", f"QTh{i}"], [f"M{sbk_}"])

                emit_S(0, sn % 2)
                for kt in range(nkt):
                    sbk = sn % 2
                    sn += 1
                    if kt + 1 < nkt:
                        emit_S(kt + 1, sn % 2)
                    for j in range(4):
                        qt = 4 * qg + j
                        if kt > 16 + qt:
                            continue
                        p = pn % NPT
                        pn += 1
                        k.act(ptb[p][:], M[sbk][:, j * 128:(j + 1) * 128], AF.Exp, [f"M{sbk}", "Btab"], [f"ptb{p}"],
                              bias=Btab[:, h, kt, qt:qt + 1], scale=1.0)
                        if kt == 16 + qt:
                            k.tt("pool", ptb[p][:], ptb[p][:], mask01[:], ALU.mult, [f"ptb{p}", "mask01"], [f"ptb{p}"])
                        oap_, ok_ = oacc(j)
                        k.mm(oap_, ptb[p][:], Vh[i][:, kt, 0:129],
                             kt == 0, kt == 16 + qt, [f"ptb{p}", f"Vh{i}"], [ok_])
                for j in range(4):
                    qt = 4 * qg + j
                    oap_, ok_ = oacc(j)
                    a = an % 2
                    an += 1
                    k.op("dve", lambda e, oap_=oap_, a=a: e.reciprocal(out=rden[a][:], in_=oap_[:, 128:129]),
                         [ok_], [f"rden{a}"])
                    k.ts("dve", atile[a][:], oap_[:, 0:128], rden[a][:, 0:1], None, ALU.mult, None,
                         [ok_, f"rden{a}"], [f"atile{a}"])
                    k.tr(TP[0][:, a * 128:(a + 1) * 128], atile[a][:], ident[:], [f"atile{a}", "ident"], [("TP0", a)])
                    k.copy("dve", attnT[:, h, qt * 128:(qt + 1) * 128], TP[0][:, a * 128:(a + 1) * 128], [("TP0", a)], [("attnT", h)])
        k.barrier()
    S_B.close()
    if debug:
        k.dma("sp", dbg_attnT, attnT[:], [("attnT", h) for h in range(NH)], [], "d_dbg")
    if upto <= 3:
        k.finish()
        return nc

    with contextlib.ExitStack() as es:
        poolT = k.sb("poolT", [128, 8, T], BF16, es)
        k.dma("sp", poolT[:], PTd, ["PTd"], [("poolT", c) for c in range(8)], "d_poolT")
        Wa = [k.sb(f"Wa{i}", [128, 8, 512], BF16, es) for i in range(2)]
        Wp = [k.sb(f"Wp{i}", [128, 8, 512], BF16, es) for i in range(2)]
        gA = [k.sb(f"gA{i}", [128, 512], BF16, es) for i in range(2)]
        gB = [k.sb(f"gB{i}", [128, 512], BF16, es) for i in range(2)]
        t1 = [k.sb(f"t1{i}", [128, 512], F32, es) for i in range(2)]
        t2 = [k.sb(f"t2{i}", [128, 512], F32, es) for i in range(2)]
        wa_v = w_battn.rearrange("(kc p) c -> p kc c", p=128)
        wp_v = w_bpool.rearrange("(kc p) c -> p kc c", p=128)

        def load_br(d4):
            i = d4 % 2
            k.dma("pool", Wa[i][:], wa_v[:, :, d4 * 512:(d4 + 1) * 512], [], [f"Wa{i}"], f"d_Wa{i}")
            k.dma("pool", Wp[i][:], wp_v[:, :, d4 * 512:(d4 + 1) * 512], [], [f"Wp{i}"], f"d_Wp{i}")

        load_br(0)
        un = 0
        for d4 in range(4):
            i = d4 % 2
            if d4 + 1 < 4:
                load_br(d4 + 1)
            for sub in range(4):
                ch = d4 * 4 + sub
                for tg in range(4):
                    u = un % 2
                    un += 1
                    k.dma("sp", gA[u][:], GT[0, ch, :, tg * 512:(tg + 1) * 512], ["GT"], [f"gA{u}"], f"d_gA{u}")
                    k.dma("sp", gB[u][:], GT[1, ch, :, tg * 512:(tg + 1) * 512], ["GT"], [f"gB{u}"], f"d_gB{u}")
                    ma, mb = 2 * u, 2 * u + 1
                    for c in range(8):
                        k.mm(M[ma][:], Wa[i][:, c, sub * 128:(sub + 1) * 128], attnT[:, c, tg * 512:(tg + 1) * 512],
                             c == 0, c == 7, [f"Wa{i}", ("attnT", c)], [f"M{ma}"])
                    for c in range(8):
                        k.mm(M[mb][:], Wp[i][:, c, sub * 128:(sub + 1) * 128], poolT[:, c, tg * 512:(tg + 1) * 512],
                             c == 0, c == 7, [f"Wp{i}", ("poolT", c)], [f"M{mb}"])
                    k.tt("dve", t1[u][:], M[ma][:], gA[u][:], ALU.mult, [f"M{ma}", f"gA{u}"], [f"t1{u}"])
                    k.tt("dve", t2[u][:], M[mb][:], gB[u][:], ALU.mult, [f"M{mb}", f"gB{u}"], [f"t2{u}"])
                    k.tt("pool", mergedT[:, ch, tg * 512:(tg + 1) * 512], t1[u][:], t2[u][:], ALU.add,
                         [f"t1{u}", f"t2{u}"], [("mergedT", ch)])
        k.barrier()
    S_A.close()
    if debug:
        k.dma("sp", dbg_mrg, mergedT[:], [("mergedT", c) for c in range(KC)], [], "d_dbg")

    with contextlib.ExitStack() as es:
        Wo = [k.sb(f"Wo{i}", [128, KC, 512], BF16, es) for i in range(2)]
        xs = [k.sb(f"xs{i}", [128, 512], F32, es) for i in range(3)]
        x1s = [k.sb(f"x1s{i}", [128, 512], F32, es) for i in range(3)]
        wo_v = w_out.rearrange("(kc p) c -> p kc c", p=128)
        k.dma("pool", Wo[0][:], wo_v[:, :, 0:512], [], ["Wo0"], "d_Wo0")
        un = 0
        for oc in range(4):
            i = oc % 2
            if oc + 1 < 4:
                k.dma("pool", Wo[1 - i][:], wo_v[:, :, (oc + 1) * 512:(oc + 2) * 512], [], [f"Wo{1 - i}"], f"d_Wo{1 - i}")
            for tt in range(NT):
                u = un % 3
                m0 = un % 4
                un += 1
                k.dma("sp", xs[u][:], x_own[tt * 128:(tt + 1) * 128, oc * 512:(oc + 1) * 512], [], [f"xs{u}"], f"d_xs{u}")
                for c in range(KC):
                    k.mm(M[m0][:], mergedT[:, c, tt * 128:(tt + 1) * 128], Wo[i][:, c, :], c == 0, c == KC - 1,
                         [f"Wo{i}", ("mergedT", c)], [f"M{m0}"])
                k.tt("dve", x1s[u][:], M[m0][:], xs[u][:], ALU.add, [f"M{m0}", f"xs{u}"], [f"x1s{u}"])
                k.dma("sp", X1[tt * 128:(tt + 1) * 128, oc * 512:(oc + 1) * 512], x1s[u][:], [f"x1s{u}"], [], f"d_x1s{u}")
        k.barrier()
    if debug:
        k.barrier()
    S_G.close()
    S_L.close()
    if upto <= 4:
        k.finish()
        return nc

    idx_all = k.sb("idx_all", [128, NT, 128], I32)
    gate_all = k.sb("gate_all", [128, NT, 128], F32)
    with contextlib.ExitStack() as es:
        xnT = k.sb("xnT", [128, KC, T], BF16, es)
        vals = k.sb("vals", [128, NT, 16, 16], F32, es)
        idxu = k.sb("idxu", [128, NT, 16, 16], U32, es)
        KsT = k.sb("KsT", [128, 16, 128], BF16, es)
        with contextlib.ExitStack() as es2:
            xin = [k.sb(f"xin{i}", [128, D], F32, es2) for i in range(2)]
            xnf = [k.sb(f"xnf{i}", [128, D], F32, es2) for i in range(2)]
            g_bc = k.sb("g_bc", [128, D], F32, es2)
            hbf = [k.sb(f"hbf{i}", [128, D], BF16, es2) for i in range(2)]
            junkb = k.sb("junkb", [128, D], BF16, es2)
            ss = [k.sb(f"ss{i}", [128, 1], F32, es2) for i in range(2)]
            kraw = k.sb("kraw", [128, 16, 128], BF16, es2)
            k.dma("sp", g_bc[:], norm2_g.partition_broadcast(128), [], ["g_bc"], "d_gbc")
            k.dma("pool", kraw[:, 0:16:2, :], sk1.rearrange("h k d -> k h d"), [], ["kraw"], "d_kraw")
            k.dma("pool", kraw[:, 1:16:2, :], sk2.rearrange("h k d -> k h d"), [], ["kraw"], "d_kraw")
            for hs in range(16):
                k.tr(TP[0][:, hs * 128:(hs + 1) * 128], kraw[:, hs, :], ident[:], ["kraw", "ident"], ["TP0"])
            k.copy("dve", KsT[:], TP[0][:].rearrange("p (c t) -> p c t", c=16), ["TP0"], ["KsT"])
            k.dma("sp", xin[0][:], X1[0:128, :], ["X1"], ["xin0"], "d_xin0")
            for tt in range(NT):
                b = tt % 2
                if tt + 1 < NT:
                    nb = (tt + 1) % 2
                    k.dma("sp", xin[nb][:], X1[(tt + 1) * 128:(tt + 2) * 128, :], ["X1"], [f"xin{nb}"], f"d_xin{nb}")
                k.memset("dve", ss[b][:], 0.0, [f"ss{b}"])
                k.act(junkb[:], xin[b][:], AF.Square, [f"xin{b}", f"ss{b}"], ["junkb", f"ss{b}"], accum_out=ss[b][:])
                k.act(ss[b][:], ss[b][:], AF.Sqrt, [f"ss{b}"], [f"ss{b}"], bias=EPS, scale=1.0 / D)
                k.op("dve", lambda e, b=b, ss=ss: e.reciprocal(out=ss[b][:], in_=ss[b][:]), [f"ss{b}"], [f"ss{b}"])
                k.stt("dve", xnf[b][:], xin[b][:], ss[b][:, 0:1], g_bc[:], ALU.mult, ALU.mult,
                      [f"xin{b}", f"ss{b}", "g_bc"], [f"xnf{b}"])
                k.copy("act", hbf[b][:], xnf[b][:], [f"xnf{b}"], [f"hbf{b}"])
                tb = 1
                for c in range(KC):
                    k.tr(TP[tb][:, c * 128:(c + 1) * 128], hbf[b][:, c * 128:(c + 1) * 128], ident[:],
                         [f"hbf{b}", "ident"], [f"TP{tb}"])
                k.copy("act", xnT[:, :, tt * 128:(tt + 1) * 128],
                       TP[tb][:].rearrange("p (c t) -> p c t", c=KC), [f"TP{tb}"], [("xnT", tt)])
        k.barrier()
        with contextlib.ExitStack() as es2:
            Wq = [k.sb(f"Wq{i}", [128, KC, 512], BF16, es2) for i in range(2)]
            qTs = [k.sb(f"qTs{i}", [128, T], BF16, es2) for i in range(2)]
            ssb = [k.sb(f"ssb{i}", [128, 128], F32, es2) for i in range(4)]
            swk = [k.sb(f"swk{i}", [128, 128], F32, es2) for i in range(2)]
            wq_v = w_query.rearrange("(kc p) c -> p kc c", p=128)
            k.dma("pool", Wq[0][:], wq_v[:, :, 0:512], [], ["Wq0"], "d_Wq0")
            mn = 0
            sn = 0
            for wl in range(4):
                i = wl % 2
                if wl + 1 < 4:
                    k.dma("pool", Wq[1 - i][:], wq_v[:, :, (wl + 1) * 512:(wl + 2) * 512], [], [f"Wq{1 - i}"], f"d_Wq{1 - i}")
                for sub in range(4):
                    hs = wl * 4 + sub
                    qb = hs % 2
                    for tg in range(4):
                        m0 = mn % 2
                        mn += 1
                        for c in range(KC):
                            k.mm(M[m0][:], Wq[i][:, c, sub * 128:(sub + 1) * 128], xnT[:, c, tg * 512:(tg + 1) * 512],
                                 c == 0, c == KC - 1, [f"Wq{i}"] + [("xnT", 4 * tg + r) for r in range(4)], [f"M{m0}"])
                        k.copy("act", qTs[qb][:, tg * 512:(tg + 1) * 512], M[m0][:], [f"M{m0}"], [(f"qTs{qb}", tg)])
                    for tt in range(NT):
                        s4 = sn % 4
                        sn += 1
                        mb_ = 2 + (s4 // 2)
                        mo = (s4 % 2) * 128
                        k.mm(M[mb_][:, mo:mo + 128], qTs[qb][:, tt * 128:(tt + 1) * 128], KsT[:, hs, :], True, True,
                             [(f"qTs{qb}", tt // 4), "KsT"], [(f"M{mb_}", s4 % 2)])
                        k.copy("act", ssb[s4][:], M[mb_][:, mo:mo + 128], [(f"M{mb_}", s4 % 2)], [f"ssb{s4}"])
                        w2 = sn % 2
                        sk_ = f"ssb{s4}"
                        vk = ("vals", tt)
                        k.op("dve", lambda e, tt=tt, hs=hs, s4=s4: e.max(out=vals[:, tt, hs, 0:8], in_=ssb[s4][:]), [sk_], [vk])
                        k.op("dve", lambda e, tt=tt, hs=hs, s4=s4: e.max_index(out=idxu[:, tt, hs, 0:8], in_max=vals[:, tt, hs, 0:8],
                                                                             in_values=ssb[s4][:]), [sk_, vk], [("idxu", tt)])
                        k.op("dve", lambda e, tt=tt, hs=hs, s4=s4, w2=w2: e.match_replace(out=swk[w2][:], in_to_replace=vals[:, tt, hs, 0:8],
                                                                                         in_values=ssb[s4][:], imm_value=NEG),
                             [sk_, vk], [f"swk{w2}"])
                        k.op("dve", lambda e, tt=tt, hs=hs, w2=w2: e.max(out=vals[:, tt, hs, 8:16], in_=swk[w2][:]), [f"swk{w2}"], [vk])
                        k.op("dve", lambda e, tt=tt, hs=hs, w2=w2: e.max_index(out=idxu[:, tt, hs, 8:16], in_max=vals[:, tt, hs, 8:16],
                                                                             in_values=swk[w2][:]), [f"swk{w2}", vk], [("idxu", tt)])
        k.barrier()
        with contextlib.ExitStack() as es2:
            idxf = k.sb("idxf", [128, NT, 16, 16], F32, es2)
            cand = [k.sb(f"cand{i}", [128, 256], F32, es2) for i in range(2)]
            cand2 = [k.sb(f"cand2{i}", [128, 256], F32, es2) for i in range(2)]
            tops = k.sb("tops", [128, NT, NH, 16], F32, es2)
            posu = k.sb("posu", [128, NT, NH, 16], U32, es2)
            pa = k.sb("pa", [128, NH * 16], I32, es2)
            pb_ = k.sb("pb_", [128, NH * 16], I32, es2)
            paf = k.sb("paf", [128, NH * 16], F32, es2)
            pbf = k.sb("pbf", [128, NH * 16], F32, es2)
            eq = k.sb("eq", [128, NH, 16, 16], F32, es2)
            If = k.sb("If", [128, NH * 16], F32, es2)
            Jf = k.sb("Jf", [128, NH * 16], F32, es2)
            iota16 = k.sb("iota16", [128, 16], F32, es2)
            esum = k.sb("esum", [128, NT * NH], F32, es2)
            k.op("pool", lambda e: e.iota(iota16[:], pattern=[[1, 16]], base=0, channel_multiplier=0,
                                          allow_small_or_imprecise_dtypes=True), [], ["iota16"])
            k.copy("dve", idxf[:].rearrange("p a b c -> p (a b c)"), idxu[:].rearrange("p a b c -> p (a b c)"),
                   [("idxu", t) for t in range(NT)], ["idxf"])
            cn = 0
            for tt in range(NT):
                for h in range(NH):
                    c = cn % 2
                    cn += 1
                    ck, c2k = f"cand{c}", f"cand2{c}"
                    vk = ("vals", tt)
                    k.tt("dve", cand[c][:].rearrange("p (a b) -> p a b", a=16),
                         vals[:, tt, 2 * h, :].unsqueeze(2).to_broadcast([128, 16, 16]),
                         vals[:, tt, 2 * h + 1, :].unsqueeze(1).to_broadcast([128, 16, 16]), ALU.add, [vk], [ck])
                    tk, pk_ = ("tops", tt), ("posu", tt)
                    k.op("dve", lambda e, tt=tt, h=h, c=c: e.max(out=tops[:, tt, h, 0:8], in_=cand[c][:]), [ck], [tk])
                    k.op("dve", lambda e, tt=tt, h=h, c=c: e.max_index(out=posu[:, tt, h, 0:8], in_max=tops[:, tt, h, 0:8],
                                                                     in_values=cand[c][:]), [ck, tk], [pk_])
                    k.op("dve", lambda e, tt=tt, h=h, c=c: e.match_replace(out=cand2[c][:], in_to_replace=tops[:, tt, h, 0:8],
                                                                         in_values=cand[c][:], imm_value=NEG), [ck, tk], [c2k])
                    k.op("dve", lambda e, tt=tt, h=h, c=c: e.max(out=tops[:, tt, h, 8:16], in_=cand2[c][:]), [c2k], [tk])
                    k.op("dve", lambda e, tt=tt, h=h, c=c: e.max_index(out=posu[:, tt, h, 8:16], in_max=tops[:, tt, h, 8:16],
                                                                     in_values=cand2[c][:]), [c2k, tk], [pk_])
                pk_ = ("posu", tt)
                pflat = posu[:, tt].rearrange("p h k -> p (h k)").bitcast(I32)
                k.ts("dve", pa[:], pflat, 4, None, ALU.logical_shift_right, None, [pk_], ["pa"])
                k.ts("dve", pb_[:], pflat, 15, None, ALU.bitwise_and, None, [pk_], ["pb_"])
                k.copy("dve", paf[:], pa[:], ["pa"], ["paf"])
                k.copy("dve", pbf[:], pb_[:], ["pb_"], ["pbf"])
                for (pf, pfk, side, dst, dk) in ((paf, "paf", 0, If, "If"), (pbf, "pbf", 1, Jf, "Jf")):
                    k.tt("dve", eq[:], iota16[:].unsqueeze(1).unsqueeze(1).to_broadcast([128, NH, 16, 16]),
                         pf[:].rearrange("p (h k) -> p h k", h=NH).unsqueeze(3).to_broadcast([128, NH, 16, 16]),
                         ALU.is_equal, ["iota16", pfk], ["eq"])
                    k.tt("dve", eq[:], eq[:], idxf[:, tt, side:16:2, :].unsqueeze(2).to_broadcast([128, NH, 16, 16]),
                         ALU.mult, ["eq", "idxf"], ["eq"])
                    k.op("dve", lambda e, dst=dst: e.tensor_reduce(out=dst[:], in_=eq[:].rearrange("p h k a -> p (h k) a"),
                                                                  axis=AX.X, op=ALU.add), ["eq"], [dk])
                k.stt("dve", If[:], If[:], 128.0, Jf[:], ALU.mult, ALU.add, ["If", "Jf"], ["If"])
                k.copy("dve", idx_all[:, tt, :], If[:], ["If"], [("idx_all", tt)])
            alltops = [("tops", t) for t in range(NT)]
            tflat = tops[:].rearrange("p t h k -> p (t h) k")
            gflat = gate_all[:].rearrange("p t (h k) -> p (t h) k", k=16)
            k.tt("dve", gflat, tflat, tflat[:, :, 0:1].to_broadcast([128, NT * NH, 16]),
                 ALU.subtract, alltops, ["gate_all"])
            k.act(gflat, gflat, AF.Exp, ["gate_all"], ["gate_all"])
            k.op("dve", lambda e: e.tensor_reduce(out=esum[:], in_=gflat, axis=AX.X, op=ALU.add), ["gate_all"], ["esum"])
            k.op("dve", lambda e: e.reciprocal(out=esum[:], in_=esum[:]), ["esum"], ["esum"])
            k.tt("dve", gflat, gflat, esum[:].unsqueeze(2).to_broadcast([128, NT * NH, 16]), ALU.mult,
                 ["gate_all", "esum"], ["gate_all"])
        k.barrier()
    if debug:
        k.dma("sp", dbg_idx, idx_all[:], [("idx_all", t) for t in range(NT)], [], "d_dbg")
        k.dma("sp", dbg_gate, gate_all[:], ["gate_all"], [], "d_dbg")
    if upto <= 5:
        k.finish()
        return nc

    with contextlib.ExitStack() as es:
        NB = PEER_NB
        xnf = [k.sb(f"pxn{i}", [128, D], F32, es) for i in range(2)]
        acc = [k.sb(f"acc{i}", [128, D], F32, es) for i in range(2)]
        ub = [k.sb(f"ub{i}", [128, D], GDT, es) for i in range(NB)]
        vb = [k.sb(f"vb{i}", [128, D], GDT, es) for i in range(NB)]
        xnb = [k.sb(f"xnb{i}", [128, D], BF16, es) for i in range(2)] if BF16_TABLES else xnf
        junk = k.sb("pjunk", [128, D], BF16, es)
        g2_bc = k.sb("g2_bc", [128, D], F32, es)
        pss = [k.sb(f"pss{i}", [128, 1], F32, es) for i in range(2)]
        k.dma("sp", g2_bc[:], norm2_g.partition_broadcast(128), [], ["g2_bc"], "d_g2bc")
        a_t = [k.sb(f"a_t{i}", [128, 128], F32, es) for i in range(2)]
        z_t = [k.sb(f"z_t{i}", [128, 128], F32, es) for i in range(2)]
        hv = [k.sb(f"hv{i}", [128, 128], F32, es) for i in range(2)]
        un = 0
        vn = 0
        for tt in range(PEER_TILES):
            b = tt % 2
            k.dma("sp", acc[b][:], X1[tt * 128:(tt + 1) * 128, :], ["X1"], [f"acc{b}"], f"d_acc{b}")
            k.memset("dve", pss[b][:], 0.0, [f"pss{b}"])
            k.act(junk[:], acc[b][:], AF.Square, [f"acc{b}", f"pss{b}"], ["pjunk", f"pss{b}"], accum_out=pss[b][:])
            k.act(pss[b][:], pss[b][:], AF.Sqrt, [f"pss{b}"], [f"pss{b}"], bias=EPS, scale=1.0 / D)
            k.op("dve", lambda e, b=b: e.reciprocal(out=pss[b][:], in_=pss[b][:]), [f"pss{b}"], [f"pss{b}"])
            k.stt("dve", xnf[b][:], acc[b][:], pss[b][:, 0:1], g2_bc[:], ALU.mult, ALU.mult,
                  [f"acc{b}", f"pss{b}", "g2_bc"], [f"pxn{b}"])
            if BF16_TABLES:
                k.copy("act", xnb[b][:], xnf[b][:], [f"pxn{b}"], [f"xnb{b}"])
            k.memset("dve", a_t[b][:], 0.0, [f"a_t{b}"])
            for s in range(128):
                u = un % NB
                un += 1
                k.op("pool", lambda e, u=u, tt=tt, s=s: e.indirect_dma_start(
                    out=ub[u][:], out_offset=None, in_=exp_u_bf,
                    in_offset=bass.IndirectOffsetOnAxis(ap=idx_all[:, tt, s:s + 1], axis=0)),
                    [("idx_all", tt)], [f"ub{u}"], dma_sem=f"d_ub{u}")
                k.stt("dve", junk[:], ub[u][:], 1.0, xnb[b][:], ALU.mult, ALU.mult, [f"ub{u}", f"xnb{b}", f"pxn{b}", f"a_t{b}"],
                      ["pjunk", f"a_t{b}"], accum_out=a_t[b][:, s:s + 1])
            k.tt("dve", z_t[b][:], a_t[b][:], a_t[b][:], ALU.mult, [f"a_t{b}"], [f"z_t{b}"])
            k.ts("dve", z_t[b][:], z_t[b][:], 0.044715, 1.0, ALU.mult, ALU.add, [f"z_t{b}"], [f"z_t{b}"])
            k.tt("dve", z_t[b][:], z_t[b][:], a_t[b][:], ALU.mult, [f"z_t{b}", f"a_t{b}"], [f"z_t{b}"])
            k.act(z_t[b][:], z_t[b][:], AF.Sigmoid, [f"z_t{b}"], [f"z_t{b}"], scale=1.5957691216057308)
            k.tt("dve", hv[b][:], z_t[b][:], a_t[b][:], ALU.mult, [f"z_t{b}", f"a_t{b}"], [f"hv{b}"])
            k.tt("dve", hv[b][:], hv[b][:], gate_all[:, tt, :], ALU.mult, [f"hv{b}", "gate_all"], [f"hv{b}"])
            for s in range(128):
                v = vn % NB
                vn += 1
                k.op("pool", lambda e, v=v, tt=tt, s=s: e.indirect_dma_start(
                    out=vb[v][:], out_offset=None, in_=exp_v_bf,
                    in_offset=bass.IndirectOffsetOnAxis(ap=idx_all[:, tt, s:s + 1], axis=0)),
                    [("idx_all", tt)], [f"vb{v}"], dma_sem=f"d_vb{v}")
                k.stt("dve", acc[b][:], vb[v][:], hv[b][:, s:s + 1], acc[b][:], ALU.mult, ALU.add,
                      [f"vb{v}", f"hv{b}", f"acc{b}"], [f"acc{b}"])
            k.dma("sp", out_d[tt * 128:(tt + 1) * 128, :], acc[b][:], [f"acc{b}"], ["out"], f"d_out{b}")
    k.finish()
    return nc


_CACHE = {}


def _host_inputs(inputs):
    f = lambda a: np.ascontiguousarray(np.asarray(a, dtype=np.float32))
    x = f(inputs["x"])
    shared = {
        "norm1_g": f(inputs["norm1_g"]).reshape(1, D),
        "w_in": f(inputs["w_in"]).reshape(D, IN_COLS),
        "forget_bias": f(inputs["forget_bias"]).reshape(NH, 1),
        "q_norm_g": f(inputs["q_norm_g"]).reshape(128, 1),
        "k_norm_g": f(inputs["k_norm_g"]).reshape(128, 1),
        "pool_group_w": f(inputs["pool_group_w"]).reshape(4, 256, 256),
        "pool_scale": f(inputs["pool_scale"]).reshape(8, 128),
        "w_branch_attn": f(inputs["w_branch_attn"]).reshape(1024, D),
        "w_branch_pool": f(inputs["w_branch_pool"]).reshape(1024, D),
        "w_out": f(inputs["w_out"]).reshape(D, D),
        "norm2_g": f(inputs["norm2_g"]).reshape(1, D),
        "peer_w_query": f(inputs["peer_w_query"]).reshape(D, D),
        "peer_sub_keys_1": f(inputs["peer_sub_keys_1"]).reshape(NH, 128, 128),
        "peer_sub_keys_2": f(inputs["peer_sub_keys_2"]).reshape(NH, 128, 128),
        "peer_expert_u": f(inputs["peer_expert_u"]).reshape(16384, D),
        "peer_expert_v": f(inputs["peer_expert_v"]).reshape(16384, D),
    }
    zeros = np.zeros((T, D), np.float32)
    in_maps = []
    for c in range(8):
        b, half = c // 2, c % 2
        m = dict(shared)
        m["x_own"] = np.ascontiguousarray(x[b, half * T:(half + 1) * T])
        m["x_prev"] = np.ascontiguousarray(x[b, 0:T]) if half == 1 else zeros
        meta = np.zeros((128, 2), np.float32)
        meta[:, 0] = float(half)
        meta[:, 1] = float(half * T)
        m["meta"] = meta
        tag = np.full((1, D), float(c), np.float32)
        m["peer_expert_u"] = np.concatenate([shared["peer_expert_u"], tag], axis=0)
        m["peer_expert_v"] = np.concatenate([shared["peer_expert_v"], tag], axis=0)
        in_maps.append(m)
    return in_maps


def kernel(**inputs):
    if "nc" not in _CACHE:
        _CACHE["nc"] = build_program()
    nc = _CACHE["nc"]
    in_maps = _host_inputs(inputs)
    res = run_bass_kernel_spmd(nc, in_maps, core_ids=list(range(8)), trace=True)
    out = np.empty((4, 4096, D), np.float32)
    for c in range(8):
        b, half = c // 2, c % 2
        out[b, half * T:(half + 1) * T] = res.results[c]["out"]
    return out
```
